# Optimizing a Trainium2 kernel written in Bass

```python
import math
import jax, jax.numpy as jnp
from jax import lax
import numpy as np

D_MODEL = 4096
BATCH = 4
SEQ = 4096
DEPTH = 2

N_HEADS = 16
N_KV_HEADS = 4
HEAD_DIM = 128
D_ATTN = N_HEADS * HEAD_DIM
D_KV = N_KV_HEADS * HEAD_DIM
IDX_HEADS = 32
IDX_DIM = 64
MAX_TOPK = 256
Q_BLOCK = 128
ROPE_THETA = 10000.0
D_SSD = D_MODEL
SSD_HEAD_DIM = 64
SSD_HEADS = D_SSD // SSD_HEAD_DIM
SSD_GROUPS = 8
SSD_STATE = 128
CONV_WIDTH = 4
SSD_CHUNK = 128
CONV_CH = D_SSD + 2 * SSD_GROUPS * SSD_STATE
D_FF = 8192
EPS = 1e-6

SPLIT_SIZES = (D_ATTN, D_KV, D_KV, IDX_HEADS * IDX_DIM, IDX_DIM, IDX_HEADS,
               D_SSD, D_SSD, SSD_GROUPS * SSD_STATE, SSD_GROUPS * SSD_STATE, SSD_HEADS,
               D_MODEL, D_MODEL)
N_IN = (D_ATTN + 2 * D_KV + IDX_HEADS * IDX_DIM + IDX_DIM + IDX_HEADS
        + 2 * D_SSD + 2 * SSD_GROUPS * SSD_STATE + SSD_HEADS + 2 * D_MODEL)

kernel_name = "hybrid_dsa_ssd_macaron_sandwich"


def _split_points(sizes):
    return [int(s) for s in np.cumsum(sizes)[:-1]]


def rms_norm(x, g):
    xf = x.astype(jnp.float32)
    y = xf * lax.rsqrt(jnp.mean(xf * xf, axis=-1, keepdims=True) + EPS)
    return (y * g.astype(jnp.float32)).astype(x.dtype)


def layer_norm(x, g, b):
    xf = x.astype(jnp.float32)
    mu = jnp.mean(xf, axis=-1, keepdims=True)
    var = jnp.mean(jnp.square(xf - mu), axis=-1, keepdims=True)
    y = (xf - mu) * lax.rsqrt(var + EPS) * g.astype(jnp.float32) + b.astype(jnp.float32)
    return y.astype(x.dtype)


def rope(x, positions):
    d = x.shape[-1]
    half = d // 2
    inv_freq = 1.0 / (ROPE_THETA ** (jnp.arange(half, dtype=jnp.float32) * (2.0 / d)))
    ang = positions.astype(jnp.float32)[..., None] * inv_freq
    cos = jnp.cos(ang)[:, :, None, :]
    sin = jnp.sin(ang)[:, :, None, :]
    xf = x.astype(jnp.float32)
    x1, x2 = xf[..., :half], xf[..., half:]
    return jnp.concatenate([x1 * cos - x2 * sin, x2 * cos + x1 * sin], axis=-1).astype(x.dtype)


def swiglu(x, w13, w2):
    a, b = jnp.split(x @ w13, 2, axis=-1)
    return (jax.nn.silu(a) * b) @ w2


def causal_depthwise_conv(x, w, b):
    c = x.shape[-1]
    out = lax.conv_general_dilated(
        x, w[:, None, :].astype(x.dtype), window_strides=(1,),
        padding=[(CONV_WIDTH - 1, 0)], dimension_numbers=("NWC", "WIO", "NWC"),
        feature_group_count=c)
    return out + b


def dsa_attention(q, k, v, qi, ki, wi, topk):
    bsz, s_len = q.shape[0], q.shape[1]
    nb = s_len // Q_BLOCK
    grp = N_HEADS // N_KV_HEADS
    scale = HEAD_DIM ** -0.5
    key_pos = jnp.arange(s_len)
    bidx = jnp.arange(bsz)[:, None, None]

    def to_blocks(t):
        return t.reshape(bsz, nb, Q_BLOCK, *t.shape[2:]).swapaxes(0, 1)

    def block(args):
        qb, qib, wib, blk = args
        qpos = blk * Q_BLOCK + jnp.arange(Q_BLOCK)
        causal = key_pos[None, :] <= qpos[:, None]
        dots = jnp.einsum('bqhd,bsd->bqhs', qib, ki)
        score = jnp.einsum('bqhs,bqh->bqs', jax.nn.relu(dots).astype(jnp.float32),
                           wib.astype(jnp.float32))
        score = jnp.where(causal[None], score, -jnp.inf)
        _, sel = lax.top_k(score, topk)
        valid = sel <= qpos[None, :, None]
        ks = k[bidx, sel]
        vs = v[bidx, sel]
        qg = qb.reshape(bsz, Q_BLOCK, N_KV_HEADS, grp, HEAD_DIM)
        logits = jnp.einsum('bqhgd,bqkhd->bqhgk', qg, ks).astype(jnp.float32) * scale
        logits = jnp.where(valid[:, :, None, None, :], logits, -jnp.inf)
        p = jax.nn.softmax(logits, axis=-1).astype(v.dtype)
        o = jnp.einsum('bqhgk,bqkhd->bqhgd', p, vs)
        return o.reshape(bsz, Q_BLOCK, N_HEADS * HEAD_DIM)

    out = lax.map(block, (to_blocks(q), to_blocks(qi), to_blocks(wi), jnp.arange(nb)))
    return out.swapaxes(0, 1).reshape(bsz, s_len, N_HEADS * HEAD_DIM)


def ssd_scan(x, dt, a, bm, cm, d_skip):
    bsz, l_len, h, p = x.shape
    g, n = bm.shape[2], bm.shape[3]
    r = h // g
    nc = l_len // SSD_CHUNK
    cl = SSD_CHUNK
    xf = x.astype(jnp.float32)
    xdt = (xf * dt[..., None]).reshape(bsz, nc, cl, g, r, p)
    adt = (dt * a).reshape(bsz, nc, cl, g, r).transpose(0, 3, 4, 1, 2)
    a_cs = jnp.cumsum(adt, axis=-1)
    bc = bm.astype(jnp.float32).reshape(bsz, nc, cl, g, n)
    cc = cm.astype(jnp.float32).reshape(bsz, nc, cl, g, n)
    tri = jnp.tril(jnp.ones((cl, cl), dtype=bool))
    seg = a_cs[..., :, None] - a_cs[..., None, :]
    lmat = jnp.exp(jnp.where(tri, seg, -jnp.inf))
    cb = jnp.einsum('bclgn,bcsgn->bgcls', cc, bc)
    y_diag = jnp.einsum('bgrcls,bcsgrp->bclgrp', cb[:, :, None] * lmat, xdt)
    decay_states = jnp.exp(a_cs[..., -1:] - a_cs).transpose(0, 3, 4, 1, 2)
    states = jnp.einsum('bclgn,bclgrp->bcgrpn', bc, xdt * decay_states[..., None])
    chunk_decay = jnp.exp(a_cs[..., -1])

    def step(hs, inp):
        s_c, d_c = inp
        return hs * d_c[..., None, None] + s_c, hs

    h0 = jnp.zeros((bsz, g, r, p, n), jnp.float32)
    _, prev = lax.scan(step, h0, (jnp.moveaxis(states, 1, 0), jnp.moveaxis(chunk_decay, -1, 0)))
    prev = jnp.moveaxis(prev, 0, 1)
    decay_out = jnp.exp(a_cs).transpose(0, 3, 4, 1, 2)
    y_off = jnp.einsum('bclgn,bcgrpn->bclgrp', cc, prev) * decay_out[..., None]
    y = (y_diag + y_off).reshape(bsz, l_len, h, p) + xf * d_skip.astype(jnp.float32)[:, None]
    return y.astype(x.dtype)


def hybrid_mixer(hn, positions, w_in, idx_ln_g, idx_ln_b, conv_w, conv_b, dt_bias, a_log,
                 d_skip, ssd_norm_g, w_attn_o, w_ssd_o, w_out):
    bsz, s_len, _ = hn.shape
    proj = hn @ w_in
    (q, k, v, qi, ki, wi, z, xs, bs, cs, dt, g_attn, g_ssd) = jnp.split(
        proj, _split_points(SPLIT_SIZES), axis=-1)
    q = rope(q.reshape(bsz, s_len, N_HEADS, HEAD_DIM), positions)
    k = rope(k.reshape(bsz, s_len, N_KV_HEADS, HEAD_DIM), positions)
    v = v.reshape(bsz, s_len, N_KV_HEADS, HEAD_DIM)
    qi = rope(qi.reshape(bsz, s_len, IDX_HEADS, IDX_DIM), positions)
    ki = rope(layer_norm(ki, idx_ln_g, idx_ln_b)[:, :, None, :], positions)[:, :, 0]
    wi = wi * (IDX_HEADS ** -0.5 * IDX_DIM ** -0.5)
    topk = min(MAX_TOPK, s_len // 4)
    o_attn = dsa_attention(q, k, v, qi, ki, wi, topk)
    xbc = jax.nn.silu(causal_depthwise_conv(jnp.concatenate([xs, bs, cs], axis=-1), conv_w, conv_b))
    xs, bs, cs = jnp.split(xbc, _split_points((D_SSD, SSD_GROUPS * SSD_STATE, SSD_GROUPS * SSD_STATE)), axis=-1)
    dt = jax.nn.softplus(dt.astype(jnp.float32) + dt_bias.astype(jnp.float32))
    a = -jnp.exp(a_log.astype(jnp.float32))
    y = ssd_scan(xs.reshape(bsz, s_len, SSD_HEADS, SSD_HEAD_DIM), dt, a,
                 bs.reshape(bsz, s_len, SSD_GROUPS, SSD_STATE),
                 cs.reshape(bsz, s_len, SSD_GROUPS, SSD_STATE), d_skip)
    y = y.reshape(bsz, s_len, D_SSD) * jax.nn.silu(z)
    y = rms_norm(y.reshape(bsz, s_len, SSD_GROUPS, D_SSD // SSD_GROUPS),
                 ssd_norm_g.reshape(SSD_GROUPS, D_SSD // SSD_GROUPS)).reshape(bsz, s_len, D_SSD)
    merged = jax.nn.sigmoid(g_attn) * (o_attn @ w_attn_o) + jax.nn.sigmoid(g_ssd) * (y @ w_ssd_o)
    return merged @ w_out


def setup_inputs(seed: int = 0) -> dict:
    key = jax.random.key(seed)
    ks = jax.random.split(key, 26)
    L, D = DEPTH, D_MODEL

    def normal(k, shape, scale):
        return jax.random.normal(k, shape, jnp.float32) * scale

    def gain(k, shape):
        return 1.0 + 0.02 * jax.random.normal(k, shape, jnp.float32)

    dt0 = jnp.exp(jax.random.uniform(ks[11], (L, SSD_HEADS), jnp.float32,
                                     math.log(1e-3), math.log(1e-1)))
    return {
        "x": normal(ks[0], (BATCH, SEQ, D), 1.0),
        "positions": jnp.broadcast_to(jnp.arange(SEQ, dtype=jnp.int32)[None, :], (BATCH, SEQ)),
        "ffn1_pre_g": gain(ks[1], (L, D)),
        "ffn1_w13": normal(ks[2], (L, D, 2 * D_FF), D ** -0.5),
        "ffn1_w2": normal(ks[3], (L, D_FF, D), D_FF ** -0.5),
        "ffn1_post_g": gain(ks[4], (L, D)),
        "mix_pre_g": gain(ks[5], (L, D)),
        "w_in": normal(ks[6], (L, D, N_IN), D ** -0.5),
        "idx_ln_g": gain(ks[7], (L, IDX_DIM)),
        "idx_ln_b": normal(ks[8], (L, IDX_DIM), 0.01),
        "conv_w": normal(ks[9], (L, CONV_WIDTH, CONV_CH), CONV_WIDTH ** -0.5),
        "conv_b": normal(ks[10], (L, CONV_CH), 0.01),
        "dt_bias": dt0 + jnp.log(-jnp.expm1(-dt0)),
        "a_log": jnp.log(jax.random.uniform(ks[12], (L, SSD_HEADS), jnp.float32, 1.0, 16.0)),
        "d_skip": gain(ks[13], (L, SSD_HEADS)),
        "ssd_norm_g": gain(ks[14], (L, D_SSD)),
        "w_attn_o": normal(ks[15], (L, D_ATTN, D), D_ATTN ** -0.5),
        "w_ssd_o": normal(ks[16], (L, D_SSD, D), D_SSD ** -0.5),
        "w_out": normal(ks[17], (L, D, D), D ** -0.5),
        "mix_post_g": gain(ks[18], (L, D)),
        "ffn2_pre_g": gain(ks[19], (L, D)),
        "ffn2_w13": normal(ks[20], (L, D, 2 * D_FF), D ** -0.5),
        "ffn2_w2": normal(ks[21], (L, D_FF, D), D_FF ** -0.5),
        "ffn2_post_g": gain(ks[22], (L, D)),
    }


def reference(x, positions, ffn1_pre_g, ffn1_w13, ffn1_w2, ffn1_post_g, mix_pre_g, w_in,
              idx_ln_g, idx_ln_b, conv_w, conv_b, dt_bias, a_log, d_skip, ssd_norm_g,
              w_attn_o, w_ssd_o, w_out, mix_post_g, ffn2_pre_g, ffn2_w13, ffn2_w2, ffn2_post_g):
    for l in range(DEPTH):
        x = x + 0.5 * rms_norm(swiglu(rms_norm(x, ffn1_pre_g[l]), ffn1_w13[l], ffn1_w2[l]),
                               ffn1_post_g[l])
        m = hybrid_mixer(rms_norm(x, mix_pre_g[l]), positions, w_in[l], idx_ln_g[l], idx_ln_b[l],
                         conv_w[l], conv_b[l], dt_bias[l], a_log[l], d_skip[l], ssd_norm_g[l],
                         w_attn_o[l], w_ssd_o[l], w_out[l])
        x = x + rms_norm(m, mix_post_g[l])
        x = x + 0.5 * rms_norm(swiglu(rms_norm(x, ffn2_pre_g[l]), ffn2_w13[l], ffn2_w2[l]),
                               ffn2_post_g[l])
    return x
```

```python
import math
import numpy as np
import concourse.bass as bass
import concourse.mybir as mybir
from contextlib import ExitStack

F32 = mybir.dt.float32
F32R = mybir.dt.float32r
I32 = mybir.dt.int32
AF = mybir.ActivationFunctionType
ALU = mybir.AluOpType
AX = mybir.AxisListType

ENGS = ("pe", "act", "dve", "pool", "sp")


class Prog:
    def __init__(self, nc, stack, n_dma_slots=16):
        self.nc = nc
        self.stack = stack
        self.streams = {e: [] for e in ENGS}
        self.cnt = {e: 0 for e in ENGS}
        self.sem = {e: stack.enter_context(nc.semaphore("sem_" + e)) for e in ENGS}
        self.waited = {e: {} for e in ENGS}
        self.res = {}
        self.nslots = n_dma_slots
        self.dsem = {}
        self.dcnt = {}
        for q in ("sp", "pool", "act"):
            self.dsem[q] = [stack.enter_context(nc.semaphore("dsem_%s_%d" % (q, i)))
                            for i in range(n_dma_slots)]
            self.dcnt[q] = 0
        self.nwaits = 0
        self.nops = 0

    def sbuf(self, name, shape, dtype=F32):
        return self.stack.enter_context(self.nc.sbuf_tensor("sb_" + name, list(shape), dtype))

    def psum(self, name, shape, dtype=F32):
        return self.stack.enter_context(self.nc.psum_tensor("ps_" + name, list(shape), dtype))

    def _r(self, key):
        r = self.res.get(key)
        if r is None:
            r = self.res[key] = [None, []]
        return r

    def _need(self, eng, tok, same_ok):
        if tok is None:
            return
        if tok[0] == "e":
            _, e2, idx = tok
            if e2 == eng and same_ok:
                return
            key = e2
            val = idx
            sem = self.sem[e2]
        else:
            _, q, slot, val = tok
            key = (q, slot)
            sem = self.dsem[q][slot]
        if self.waited[eng].get(key, 0) >= val:
            return
        self.waited[eng][key] = val
        self.nwaits += 1
        self.streams[eng].append(lambda e, sem=sem, val=val: e.wait_ge(sem, val))

    def _deps(self, eng, reads, writes):
        pe = (eng == "pe")
        for k in reads:
            r = self._r(k)
            self._need(eng, r[0], same_ok=pe)
        for k in writes:
            r = self._r(k)
            self._need(eng, r[0], same_ok=pe)
            for t in r[1]:
                self._need(eng, t, same_ok=True)

    def _commit(self, tok, reads, writes):
        for k in reads:
            self._r(k)[1].append(tok)
        for k in writes:
            r = self._r(k)
            r[0] = tok
            r[1] = []

    def op(self, eng, fn, reads=(), writes=()):
        self._deps(eng, reads, writes)
        self.cnt[eng] += 1
        idx = self.cnt[eng]
        sem = self.sem[eng]
        self.streams[eng].append(lambda e, fn=fn, sem=sem: fn(e).then_inc(sem, 1))
        self._commit(("e", eng, idx), reads, writes)
        self.nops += 1

    def dma(self, q, out, in_, reads=(), writes=(), **kw):
        j = self.dcnt[q]
        self.dcnt[q] += 1
        slot = j % self.nslots
        use = j // self.nslots
        if use > 0:
            self._need(q, ("d", q, slot, 16 * use), same_ok=False)
        self._deps(q, reads, writes)
        sem = self.dsem[q][slot]
        self.streams[q].append(
            lambda e, out=out, in_=in_, sem=sem, kw=kw: e.dma_start(out=out, in_=in_, **kw).then_inc(sem, 16))
        self._commit(("d", q, slot, 16 * (use + 1)), reads, writes)
        self.nops += 1

    def wait_all(self, eng, keys):
        for k in keys:
            self._need(eng, self._r(k)[0], same_ok=False)

    def emit(self):
        nc = self.nc
        with nc.Block() as block:
            @block.tensor
            def _(e):
                for f in self.streams["pe"]:
                    f(e)

            @block.scalar
            def _(e):
                for f in self.streams["act"]:
                    f(e)

            @block.vector
            def _(e):
                for f in self.streams["dve"]:
                    f(e)

            @block.gpsimd
            def _(e):
                for f in self.streams["pool"]:
                    f(e)

            @block.sync
            def _(e):
                for f in self.streams["sp"]:
                    f(e)


def _barrier(self):
    toks = [("e", e2, self.cnt[e2]) for e2 in ENGS if self.cnt[e2] > 0]
    for q in self.dsem:
        n = self.dcnt[q]
        for slot in range(min(n, self.nslots)):
            uses = (n - slot + self.nslots - 1) // self.nslots
            toks.append(("d", q, slot, 16 * uses))
    for e in ENGS:
        for t in toks:
            if t[0] == "e" and t[1] == e:
                continue
            self._need(e, t, same_ok=False)
    self.res = {}


Prog.barrier = _barrier


BF16 = mybir.dt.bfloat16
T = 256
NT = 2
EPS = 1e-6
THETA = 10000.0
NEG = -1.0e30


class Cfg:
    def __init__(s, D=4096, S=4096, NH=16, NKV=4, IH=32, DS=4096, SG=8, TOPK=256):
        s.D, s.S, s.NH, s.NKV, s.IH, s.DS, s.SG, s.TOPK = D, S, NH, NKV, IH, DS, SG, TOPK
        s.HD, s.ID, s.SP, s.SN = 128, 64, 64, 128
        s.SH = DS // 64
        s.R = s.SH // SG
        s.RP = s.R * 64
        s.DA, s.DKV, s.GN = NH * 128, NKV * 128, SG * 128
        s.SO = S // 2
        s.DC = D // 128
        s.NQB = s.SO // 128
        sizes = (s.DA, s.DKV, s.DKV, IH * 64, 64, IH, DS, DS, s.GN, s.GN, s.SH, D, D)
        offs = np.concatenate([[0], np.cumsum(sizes)])
        (s.OQ, s.OK, s.OV, s.OQI, s.OKI, s.OWI, s.OZ, s.OXS, s.OBS, s.OCS, s.ODT, s.OGA, s.OGS, s.NIN) = [int(o) for o in offs]
        s.NCC = (DS + 2 * s.GN) // 128
        s.GRPQ = NH // NKV


def host_consts(cfg, h):
    c = {}
    c["ident"] = np.eye(128, dtype=np.float32)
    R = np.zeros((128, 128), np.float32)
    for i in range(64):
        R[i, i + 64] = -1.0
        R[i + 64, i] = 1.0
    c["rt128"] = np.ascontiguousarray(R.T)
    R2 = np.zeros((128, 128), np.float32)
    for b in range(2):
        for i in range(32):
            R2[64 * b + i, 64 * b + i + 32] = -1.0
            R2[64 * b + i + 32, 64 * b + i] = 1.0
    c["rt64"] = np.ascontiguousarray(R2.T)
    p = np.arange(128)
    inv128 = (1.0 / (THETA ** ((p % 64).astype(np.float32) * np.float32(2.0 / 128)))).astype(np.float32)
    inv64 = (1.0 / (THETA ** ((p % 32).astype(np.float32) * np.float32(2.0 / 64)))).astype(np.float32)
    c["invc"] = np.stack([inv128, inv64], 1).astype(np.float32)
    i32 = np.arange(32)
    invrow = (1.0 / (THETA ** (i32.astype(np.float32) * np.float32(2.0 / 64)))).astype(np.float32)
    c["invrow"] = np.ascontiguousarray(np.broadcast_to(invrow, (128, 32)))
    tri = (p[:, None] <= p[None, :]).astype(np.float32)
    c["tri"] = tri
    c["ones"] = np.ones((128, 128), np.float32)
    c["iota"] = np.ascontiguousarray(np.broadcast_to(np.arange(cfg.S, dtype=np.float32), (128, cfg.S)))
    own_blocks = 2 * np.arange(cfg.NQB) + h
    qpos = own_blocks[None, :] * 128 + p[:, None]
    c["qpos"] = qpos.astype(np.float32)
    c["sel"] = np.ascontiguousarray(np.broadcast_to(np.array([1.0 - h, float(h)], np.float32), (128, 2)))
    return c


def bc(v, n=128):
    return np.ascontiguousarray(np.broadcast_to(np.asarray(v, np.float32).reshape(1, -1), (n, v.size)))


def cols(v):
    v = np.asarray(v, np.float32)
    return np.ascontiguousarray(v.reshape(-1, 128).T)


def host_inputs(cfg, h, x1_all, pos_all, mix_pre_g, w_in, idx_ln_g, idx_ln_b, conv_w, conv_b, dt_bias, a_log,
                d_skip, ssd_norm_g, w_attn_o, w_ssd_o, w_out, mix_post_g):
    S, SO = cfg.S, cfg.SO
    own = np.concatenate([np.arange(128) + 128 * (2 * j + h) for j in range(cfg.NQB)])
    m = host_consts(cfg, h)
    m["xa"] = x1_all
    xo = np.ascontiguousarray(x1_all[own])
    m["xo"] = xo
    m["xor"] = xo
    m["posb_all"] = np.ascontiguousarray(np.broadcast_to(pos_all.astype(np.int32), (128, S)))
    m["posb_own"] = np.ascontiguousarray(np.broadcast_to(pos_all[own].astype(np.int32), (128, SO)))
    m["posc_all"] = np.ascontiguousarray(pos_all.astype(np.int32).reshape(S // 128, 128).T)
    m["gpre_c"] = cols(mix_pre_g)
    m["gpost_b"] = bc(mix_post_g)
    m["lng_b"] = bc(idx_ln_g)
    m["lnb_b"] = bc(idx_ln_b)
    m["convw_c"] = np.ascontiguousarray(conv_w.T.reshape(cfg.NCC, 128, 4).transpose(1, 0, 2).reshape(128, cfg.NCC * 4))
    m["convb_c"] = cols(conv_b)
    m["dtb_b"] = bc(dt_bias)
    m["alog_b"] = bc(a_log)
    m["dskip_b"] = bc(np.repeat(np.asarray(d_skip, np.float32), 64))
    m["ssdg_b"] = bc(ssd_norm_g)
    m["w_in"] = w_in
    m["w_attn_o"] = w_attn_o
    m["w_ssd_o"] = w_ssd_o
    m["w_out"] = w_out
    return m


def emit_mixer(P, nc, cfg, io, phases=(1, 2, 3, 4, 5), dbg=None):
    D, S, SO, DC = cfg.D, cfg.S, cfg.SO, cfg.DC
    NH, NKV, IH, DS, SG, SH, RP, R = cfg.NH, cfg.NKV, cfg.IH, cfg.DS, cfg.SG, cfg.SH, cfg.RP, cfg.R
    GN, NCC = cfg.GN, cfg.NCC

    def dram(name, shape, dt=F32):
        return nc.dram_tensor(name, list(shape), dt, kind="Internal").ap()

    KT = dram("s_KT", [NKV, 128, S], F32R)
    V = dram("s_V", [S, NKV * 128], F32R)
    KIT = dram("s_KIT", [128, S], F32R)
    DTs = dram("s_DT", [S, SH])
    XS = dram("s_XS", [S, DS], F32R)
    BTK = dram("s_BTK", [S, GN], F32R)
    BT = dram("s_BT", [SG, 128, S], F32R)
    CT = dram("s_CT", [SG, 128, S], F32R)
    QT = dram("s_QT", [NH, 128, SO], F32R)
    QIT = dram("s_QIT", [IH // 2, 128, SO], F32R)
    WI = dram("s_WI", [SO, IH])
    ZS = dram("s_ZS", [SO, DS])
    GA = dram("s_GA", [DC, 128, SO])
    GS = dram("s_GS", [DC, 128, SO])
    YT = dram("s_YT", [DS // 128, 128, SO], F32R)
    OT = dram("s_OT", [NH, 128, SO], F32R)

    tp = [P.psum("tp%d" % i, [128, 512]) for i in range(2)]
    mp = [P.psum("mp%d" % i, [128, 1024]) for i in range(3)]
    st8 = {"tp": 0, "mp": 0, "w": 0}

    def next_tp():
        i = st8["tp"] % 2
        st8["tp"] += 1
        return tp[i], ("tp", i)

    def next_mp():
        i = st8["mp"] % 3
        st8["mp"] += 1
        return mp[i], ("mp", i)

    ck = {}

    def const(name, shape, dt=F32, src=None):
        t = P.sbuf("c_" + name, shape, dt)
        P.dma("sp", t[:, :], (src if src is not None else io[name])[:, :], writes=["c_" + name])
        ck[name] = "c_" + name
        return t

    ident = const("ident", [128, 128])
    ones = const("ones", [128, 128])
    tri = const("tri", [128, 128])
    rt128 = const("rt128", [128, 128], F32R)
    rt64 = const("rt64", [128, 128], F32R)
    invc = const("invc", [128, 2])
    sel = const("sel", [128, 2])
    Wl = [None]

    def alloc_w():
        Wl[0] = [P.sbuf("W%d_%d" % (i, st8["w"]), [128, 512], F32R) for i in range(8)]

    def next_w():
        W = Wl[0]
        i = st8["w"] % 8
        st8["w"] += 1
        return W[i], ("W", i)

    def act_copy(out, in_, r, w, func=AF.Copy, **kw):
        P.op("act", lambda e: e.activation(out=out, in_=in_, func=func, **kw), reads=r, writes=w)

    def dve_tt(out, a, b, op, r, w):
        P.op("dve", lambda e: e.tensor_tensor(out=out, in0=a, in1=b, op=op), reads=r, writes=w)

    def dve_ts(out, a, s1, s2, op0, op1, r, w):
        if op1 is None:
            P.op("dve", lambda e: e.tensor_scalar(out=out, in0=a, scalar1=s1, scalar2=None, op0=op0), reads=r, writes=w)
        else:
            P.op("dve", lambda e: e.tensor_scalar(out=out, in0=a, scalar1=s1, scalar2=s2, op0=op0, op1=op1),
                 reads=r, writes=w)

    def dve_stt(out, a, sc, b, op0, op1, r, w):
        P.op("dve", lambda e: e.scalar_tensor_tensor(out=out, in0=a, scalar=sc, in1=b, op0=op0, op1=op1),
             reads=r, writes=w)

    def rstd_from_ss(col, key, scale, sqscale=1.0):
        dve_ts(col, col, scale, EPS, ALU.mult, ALU.add, [key], [key])
        P.op("act", lambda e: e.activation(out=col, in_=col, func=AF.Sqrt, scale=sqscale), reads=[key], writes=[key])
        P.op("dve", lambda e: e.reciprocal(out=col, in_=col), reads=[key], writes=[key])

    def sin_table(out, okey, posf, pkey, n, invcol, phase, tmp, tmpi, tkey):
        a, kf = tmp
        if phase == 0.0:
            dve_ts(a, posf, invcol, None, ALU.mult, None, [pkey], [tkey + "_a"])
        else:
            dve_ts(a, posf, invcol, phase, ALU.mult, ALU.add, [pkey], [tkey + "_a"])
        dve_ts(kf, a, 1.0 / (2 * math.pi), None, ALU.mult, None, [tkey + "_a"], [tkey + "_k"])
        P.op("dve", lambda e: e.tensor_copy(out=tmpi, in_=kf), reads=[tkey + "_k"], writes=[tkey + "_i"])
        P.op("dve", lambda e: e.tensor_copy(out=kf, in_=tmpi), reads=[tkey + "_i"], writes=[tkey + "_k"])
        dve_stt(a, kf, -2 * math.pi, a, ALU.mult, ALU.add, [tkey + "_k", tkey + "_a"], [tkey + "_a"])
        dve_ts(kf, a, math.pi, -2 * math.pi, ALU.is_gt, ALU.mult, [tkey + "_a"], [tkey + "_k"])
        dve_tt(a, a, kf, ALU.add, [tkey + "_a", tkey + "_k"], [tkey + "_a"])
        dve_ts(kf, a, -math.pi, 2 * math.pi, ALU.is_lt, ALU.mult, [tkey + "_a"], [tkey + "_k"])
        dve_tt(a, a, kf, ALU.add, [tkey + "_a", tkey + "_k"], [tkey + "_a"])
        P.op("act", lambda e: e.activation(out=out, in_=a, func=AF.Sin), reads=[tkey + "_a"], writes=[okey])

    if 1 in phases or 2 in phases:
        with ExitStack() as ph:
            old = P.stack
            P.stack = ph
            alloc_w()
            A = P.sbuf("A", [128, NT * D])
            B = P.sbuf("B", [128, DC * T], F32R)
            junk = P.sbuf("junk", [128, D], BF16)
            gpre = P.sbuf("gpre", [128, DC])
            st = P.sbuf("st", [128, 16])
            pint = P.sbuf("pint", [128, T], I32)
            posf = P.sbuf("posf", [128, T])
            tmpa = P.sbuf("tmpa", [128, T])
            tmpk = P.sbuf("tmpk", [128, T])
            tmpi = P.sbuf("tmpi", [128, T], I32)
            tabs = {n: P.sbuf("tab_" + n, [128, T]) for n in ("c128", "s128", "c64", "s64")}
            xsb = P.sbuf("xsb", [128, 4 * T], F32R)
            rop = P.sbuf("rop", [128, 4 * T], F32R)
            rt1 = P.sbuf("rt1", [128, T])
            tks = P.sbuf("tks", [128, NT * 512], F32R)
            P.dma("sp", gpre[:, :], io["gpre_c"][:, :], writes=["gpre"])

            def front(xsrc, t0):
                for k in range(NT):
                    Ak = A[:, k * D:(k + 1) * D]
                    P.dma("sp", Ak, xsrc[t0 + 128 * k:t0 + 128 * (k + 1), :], writes=[("A", k)])
                    P.op("dve", lambda e, k=k: e.memset(st[:, k:k + 1], 0.0), writes=[("st", k)])
                    P.op("act", lambda e, Ak=Ak, k=k: e.activation(out=junk[:, :], in_=Ak, func=AF.Square,
                                                                  accum_out=st[:, k:k + 1]),
                         reads=[("A", k)], writes=["junk", ("st", k)])
                    rstd_from_ss(st[:, k:k + 1], ("st", k), 1.0 / D)
                    dve_ts(Ak, Ak, st[:, k:k + 1], None, ALU.mult, None, [("A", k), ("st", k)], [("A", k)])
                for c2 in range(DC // 2):
                    ps, pk = next_tp()
                    for cc in range(2):
                        c = 2 * c2 + cc
                        for k in range(NT):
                            P.op("pe", lambda e, ps=ps, cc=cc, k=k, c=c: e.transpose(
                                out=ps[:, (cc * NT + k) * 128:(cc * NT + k + 1) * 128],
                                in_=A[:, k * D + c * 128:k * D + (c + 1) * 128], identity=ident[:, :]),
                                 reads=[("A", k), ck["ident"]], writes=[pk])
                    for cc in range(2):
                        c = 2 * c2 + cc
                        dve_ts(B[:, c * T:(c + 1) * T], ps[:, cc * T:(cc + 1) * T], gpre[:, c:c + 1], None,
                               ALU.mult, None, [pk, "gpre"], [("B", c)])

            def make_tables(possrc, t0, which):
                P.dma("sp", pint[:, :], possrc[:, t0:t0 + T], writes=["pint"])
                P.op("dve", lambda e: e.tensor_copy(out=posf[:, :], in_=pint[:, :]), reads=["pint"], writes=["posf"])
                for n in which:
                    ic = invc[:, 0:1] if n.endswith("128") else invc[:, 1:2]
                    ph_ = math.pi / 2 if n[0] == "c" else 0.0
                    sin_table(tabs[n][:, :], "tab_" + n, posf[:, :], "posf", T, ic, ph_, (tmpa[:, :], tmpk[:, :]),
                              tmpi[:, :], "tt")

            def fm_group(col0, nch):
                ps, pk = next_mp()
                for dk in range(DC):
                    wb, wk = next_w()
                    P.dma("sp", wb[:, 0:128 * nch], io["w_in"][dk * 128:(dk + 1) * 128, col0:col0 + 128 * nch],
                          writes=[wk])
                    for m in range(nch):
                        P.op("pe", lambda e, ps=ps, wb=wb, m=m, dk=dk: e.matmul(
                            ps[:, m * T:(m + 1) * T], lhsT=wb[:, m * 128:(m + 1) * 128],
                            rhs=B[:, dk * T:(dk + 1) * T], start=(dk == 0 and m % 2 == 0), stop=(dk == DC - 1),
                            skip_group_check=True), reads=[wk, ("B", dk)], writes=[pk])
                return ps, pk

            def tm_block(col0, ncol):
                ps, pk = next_mp()
                for dk in range(DC):
                    wb, wk = next_w()
                    P.dma("sp", wb[:, 0:ncol], io["w_in"][dk * 128:(dk + 1) * 128, col0:col0 + ncol], writes=[wk])
                    for k in range(NT):
                        P.op("pe", lambda e, ps=ps, wb=wb, k=k, dk=dk: e.matmul(
                            ps[:, k * 512:k * 512 + ncol], lhsT=B[:, dk * T + k * 128:dk * T + (k + 1) * 128],
                            rhs=wb[:, 0:ncol], start=(dk == 0), stop=(dk == DC - 1)),
                             reads=[wk, ("B", dk)], writes=[pk])
                return ps, pk

            def rope_group(ps, pk, nch, rt, rtk, cn, sn, dst_fn):
                act_copy(xsb[:, 0:nch * T], ps[:, 0:nch * T], [pk], ["xsb"])
                for m in range(nch):
                    rp, rk = next_tp()
                    P.op("pe", lambda e, rp=rp, m=m: e.matmul(rp[:, 0:T], lhsT=rt[:, :], rhs=xsb[:, m * T:(m + 1) * T],
                                                              start=True, stop=True),
                         reads=["xsb", rtk], writes=[rk])
                    dve_tt(rt1[:, :], xsb[:, m * T:(m + 1) * T].bitcast(F32), tabs[cn][:, :], ALU.mult,
                           ["xsb", "tab_" + cn], ["rt1"])
                    dve_tt(rop[:, m * T:(m + 1) * T], rp[:, 0:T], tabs[sn][:, :], ALU.mult, [rk, "tab_" + sn],
                           [("rop", m)])
                    dve_tt(rop[:, m * T:(m + 1) * T], rop[:, m * T:(m + 1) * T].bitcast(F32), rt1[:, :], ALU.add,
                           [("rop", m), "rt1"], [("rop", m)])
                    P.dma("sp", dst_fn(m), rop[:, m * T:(m + 1) * T], reads=[("rop", m)], writes=[("scr", "rope", m)])

            if 1 in phases:
                lng = P.sbuf("lng", [128, 64])
                lnb = P.sbuf("lnb", [128, 64])
                invrow = P.sbuf("invrow", [128, 32])
                posc = P.sbuf("posc", [128, S // 128], I32)
                poscf = P.sbuf("poscf", [128, S // 128])
                dtb = P.sbuf("dtb", [128, SH])
                cw = P.sbuf("cw", [128, NCC * 4])
                cb = P.sbuf("cb", [128, NCC])
                HALO = P.sbuf("halo", [128, NCC * 3])
                cst = P.sbuf("cst", [128, 4 * (T + 3)])
                cacc = P.sbuf("cacc", [128, T])
                so4 = P.sbuf("so4", [128, 4 * T], F32R)
                kis = P.sbuf("kis", [128, 64])
                kin = P.sbuf("kin", [128, 128])
                ktab = P.sbuf("ktab", [128, 6 * 32])
                ktabi = P.sbuf("ktabi", [128, 32], I32)
                kt6 = P.sbuf("kt6", [128, 6 * 32])
                kst = P.sbuf("kst", [128, 128], F32R)
                dts = P.sbuf("dts", [128, 4 * SH])
                for nm, t_ in (("lng_b", lng), ("lnb_b", lnb), ("invrow", invrow), ("dtb_b", dtb), ("convw_c", cw),
                               ("convb_c", cb)):
                    P.dma("sp", t_[:, :], io[nm][:, :], writes=[nm])
                P.dma("sp", posc[:, :], io["posc_all"][:, :], writes=["posc"])
                P.op("dve", lambda e: e.tensor_copy(out=poscf[:, :], in_=posc[:, :]), reads=["posc"], writes=["poscf"])
                P.op("dve", lambda e: e.memset(HALO[:, :], 0.0), writes=["halo"])
                cst3 = cst[:, :].rearrange("p (m t) -> p m t", m=4)
                halo3 = HALO[:, :].rearrange("p (c k) -> p c k", k=3)
                for tt in range(S // T):
                    t0 = tt * T
                    front(io["xa"], t0)
                    make_tables(io["posb_all"], t0, ("c128", "s128"))
                    for g0 in range(0, NKV, 4):
                        n = min(4, NKV - g0)
                        ps, pk = fm_group(cfg.OK + 128 * g0, n)
                        rope_group(ps, pk, n, rt128, ck["rt128"], "c128", "s128",
                                   lambda m, g0=g0, t0=t0: KT[g0 + m, :, t0:t0 + T])
                    for c0 in range(0, NKV * 128, 512):
                        n = min(512, NKV * 128 - c0)
                        ps, pk = tm_block(cfg.OV + c0, n)
                        for k in range(NT):
                            act_copy(tks[:, k * 512:k * 512 + n], ps[:, k * 512:k * 512 + n], [pk], [("tks", k)])
                            P.dma("sp", V[t0 + 128 * k:t0 + 128 * (k + 1), c0:c0 + n], tks[:, k * 512:k * 512 + n],
                                  reads=[("tks", k)], writes=[("scr", "v", k)])
                    ps, pk = next_mp()
                    first = True
                    for dk in range(DC):
                        wb, wk = next_w()
                        P.dma("sp", wb[:, 0:64], io["w_in"][dk * 128:(dk + 1) * 128, cfg.OKI:cfg.OKI + 64], writes=[wk])
                        wb2, wk2 = next_w()
                        P.dma("sp", wb2[:, 0:SH], io["w_in"][dk * 128:(dk + 1) * 128, cfg.ODT:cfg.ODT + SH], writes=[wk2])
                        for k in range(NT):
                            lh = B[:, dk * T + k * 128:dk * T + (k + 1) * 128]
                            P.op("pe", lambda e, ps=ps, wb=wb, k=k, lh=lh, first=first, dk=dk: e.matmul(
                                ps[:, k * 64:(k + 1) * 64], lhsT=lh, rhs=wb[:, 0:64], start=first, stop=(dk == DC - 1),
                                skip_group_check=True), reads=[wk, ("B", dk)], writes=[pk])
                            first = False
                            P.op("pe", lambda e, ps=ps, wb2=wb2, k=k, lh=lh, dk=dk: e.matmul(
                                ps[:, 128 + k * SH:128 + (k + 1) * SH], lhsT=lh, rhs=wb2[:, 0:SH], start=False,
                                stop=(dk == DC - 1), skip_group_check=True), reads=[wk2, ("B", dk)], writes=[pk])
                    for k in range(NT):
                        act_copy(kis[:, :], ps[:, k * 64:(k + 1) * 64], [pk], ["kis"])
                        P.op("dve", lambda e: e.reduce_sum(out=st[:, 8:9], in_=kis[:, :], axis=AX.X), reads=["kis"],
                             writes=["st8"])
                        dve_ts(st[:, 8:9], st[:, 8:9], -1.0 / 64, None, ALU.mult, None, ["st8"], ["st8"])
                        dve_ts(kis[:, :], kis[:, :], st[:, 8:9], None, ALU.add, None, ["kis", "st8"], ["kis"])
                        P.op("dve", lambda e: e.memset(st[:, 9:10], 0.0), writes=["st9"])
                        P.op("act", lambda e: e.activation(out=junk[:, 0:64], in_=kis[:, :], func=AF.Square,
                                                           accum_out=st[:, 9:10]), reads=["kis"], writes=["junk", "st9"])
                        rstd_from_ss(st[:, 9:10], "st9", 1.0 / 64)
                        dve_stt(kis[:, :], kis[:, :], st[:, 9:10], lng[:, :], ALU.mult, ALU.mult, ["kis", "st9", "lng_b"],
                                ["kis"])
                        dve_tt(kis[:, :], kis[:, :], lnb[:, :], ALU.add, ["kis", "lnb_b"], ["kis"])
                        chunk = t0 // 128 + k
                        pcol = poscf[:, chunk:chunk + 1]
                        dve_ts(ktab[:, 0:32], invrow[:, :], pcol, None, ALU.mult, None, ["invrow", "poscf"], ["ktab0"])
                        sin_table(ktab[:, 64:96], "ktS", ktab[:, 0:32], "ktab0", 32, 1.0, 0.0,
                                  (ktab[:, 128:160], ktab[:, 160:192]), ktabi[:, :], "kt")
                        sin_table(ktab[:, 96:128], "ktC", ktab[:, 0:32], "ktab0", 32, 1.0, math.pi / 2,
                                  (ktab[:, 128:160], ktab[:, 160:192]), ktabi[:, :], "kt")
                        x1, x2 = kis[:, 0:32], kis[:, 32:64]
                        sn_, cs_ = ktab[:, 64:96], ktab[:, 96:128]
                        dve_tt(kt6[:, 0:32], x1, cs_, ALU.mult, ["kis", "ktC"], ["k6a"])
                        dve_tt(kt6[:, 32:64], x2, sn_, ALU.mult, ["kis", "ktS"], ["k6b"])
                        dve_tt(kt6[:, 64:96], x2, cs_, ALU.mult, ["kis", "ktC"], ["k6c"])
                        dve_tt(kt6[:, 96:128], x1, sn_, ALU.mult, ["kis", "ktS"], ["k6d"])
                        dve_tt(kin[:, 0:32], kt6[:, 0:32], kt6[:, 32:64], ALU.subtract, ["k6a", "k6b"], ["kin0"])
                        dve_tt(kin[:, 32:64], kt6[:, 64:96], kt6[:, 96:128], ALU.add, ["k6c", "k6d"], ["kin1"])
                        P.op("dve", lambda e: e.tensor_copy(out=kin[:, 64:128], in_=kin[:, 0:64]), reads=["kin0", "kin1"],
                             writes=["kin2"])
                        rp, rk = next_tp()
                        P.op("pe", lambda e, rp=rp: e.transpose(out=rp[:, 0:128], in_=kin[:, :], identity=ident[:, :]),
                             reads=["kin0", "kin1", "kin2", ck["ident"]], writes=[rk])
                        act_copy(kst[:, :], rp[:, 0:128], [rk], ["kst"])
                        P.dma("sp", KIT[:, t0 + 128 * k:t0 + 128 * (k + 1)], kst[:, :], reads=["kst"],
                              writes=[("scr", "kit", k)])
                        d0 = dts[:, 0:SH]
                        d1 = dts[:, SH:2 * SH]
                        d2 = dts[:, 2 * SH:3 * SH]
                        dve_tt(d0, ps[:, 128 + k * SH:128 + (k + 1) * SH], dtb[:, :], ALU.add, [pk, "dtb_b"], ["d0"])
                        dve_ts(d1, d0, -1.0, None, ALU.mult, None, ["d0"], ["d1"])
                        dve_tt(d1, d1, d0, ALU.min, ["d0", "d1"], ["d1"])
                        act_copy(d1, d1, ["d1"], ["d1"], func=AF.Exp)
                        dve_ts(d1, d1, 1.0, None, ALU.add, None, ["d1"], ["d1"])
                        act_copy(d1, d1, ["d1"], ["d1"], func=AF.Ln)
                        dve_ts(d2, d0, 0.0, None, ALU.max, None, ["d0"], ["d2"])
                        dve_tt(d2, d2, d1, ALU.add, ["d2", "d1"], ["d2"])
                        P.dma("sp", DTs[t0 + 128 * k:t0 + 128 * (k + 1), :], d2, reads=["d2"], writes=[("scr", "dt", k)])
                    cgroups = []
                    for (a_, b_) in ((0, DS // 128), (DS // 128, DS // 128 + SG), (DS // 128 + SG, NCC)):
                        for c_ in range(a_, b_, 4):
                            cgroups.append((c_, min(4, b_ - c_)))
                    for (cc0, n) in cgroups:
                        ps, pk = fm_group(cfg.OXS + 128 * cc0, n)
                        P.op("dve", lambda e, cc0=cc0, n=n: e.tensor_copy(out=cst3[:, 0:n, 0:3],
                                                                          in_=halo3[:, cc0:cc0 + n, :]),
                             reads=["halo"], writes=["cst_h"])
                        P.op("act", lambda e, ps=ps, n=n: e.activation(
                            out=cst3[:, 0:n, 3:3 + T], in_=ps[:, 0:n * T].rearrange("p (m t) -> p m t", m=n),
                            func=AF.Copy), reads=[pk], writes=["cst_b"])
                        P.op("dve", lambda e, cc0=cc0, n=n: e.tensor_copy(out=halo3[:, cc0:cc0 + n, :],
                                                                          in_=cst3[:, 0:n, T:T + 3]),
                             reads=["cst_b", "cst_h"], writes=["halo"])
                        for m in range(n):
                            cc = cc0 + m
                            base = m * (T + 3)
                            dve_ts(cacc[:, :], cst[:, base:base + T], cw[:, cc * 4:cc * 4 + 1], None, ALU.mult, None,
                                   ["cst_b", "cst_h", "convw_c"], ["cacc"])
                            for kk in range(1, 4):
                                dve_stt(cacc[:, :], cst[:, base + kk:base + kk + T], cw[:, cc * 4 + kk:cc * 4 + kk + 1],
                                        cacc[:, :], ALU.mult, ALU.add, ["cst_b", "cst_h", "cacc", "convw_c"], ["cacc"])
                            act_copy(so4[:, m * T:(m + 1) * T], cacc[:, :], ["cacc", "convb_c"], [("so4", m)],
                                     func=AF.Silu, bias=cb[:, cc:cc + 1])
                            if cc >= DS // 128:
                                gi = cc - DS // 128
                                dst = BT[gi, :, t0:t0 + T] if gi < SG else CT[gi - SG, :, t0:t0 + T]
                                P.dma("sp", dst, so4[:, m * T:(m + 1) * T], reads=[("so4", m)], writes=[("scr", "bc", m)])
                        if cc0 < DS // 128 + SG:
                            for k in range(NT):
                                rp, rk = next_tp()
                                for m in range(n):
                                    P.op("pe", lambda e, rp=rp, m=m, k=k: e.transpose(
                                        out=rp[:, m * 128:(m + 1) * 128],
                                        in_=so4[:, m * T + k * 128:m * T + (k + 1) * 128].bitcast(F32),
                                        identity=ident[:, :]), reads=[("so4", m), ck["ident"]], writes=[rk])
                                act_copy(tks[:, k * 512:k * 512 + 128 * n], rp[:, 0:128 * n], [rk], [("tks", k)])
                                if cc0 < DS // 128:
                                    dst = XS[t0 + 128 * k:t0 + 128 * (k + 1), cc0 * 128:(cc0 + n) * 128]
                                else:
                                    g0_ = cc0 - DS // 128
                                    dst = BTK[t0 + 128 * k:t0 + 128 * (k + 1), g0_ * 128:(g0_ + n) * 128]
                                P.dma("sp", dst, tks[:, k * 512:k * 512 + 128 * n], reads=[("tks", k)],
                                      writes=[("scr", "tk", k)])

            if 2 in phases:
                gst = P.sbuf("gst", [128, 4 * T])
                wis = P.sbuf("wis", [128, 2 * IH])
                zst = P.sbuf("zst", [128, NT * 512])
                for tt in range(SO // T):
                    t0 = tt * T
                    front(io["xo"], t0)
                    make_tables(io["posb_own"], t0, ("c128", "s128", "c64", "s64"))
                    for h0 in range(0, NH, 4):
                        n = min(4, NH - h0)
                        ps, pk = fm_group(cfg.OQ + 128 * h0, n)
                        rope_group(ps, pk, n, rt128, ck["rt128"], "c128", "s128",
                                   lambda m, h0=h0, t0=t0: QT[h0 + m, :, t0:t0 + T])
                    for h0 in range(0, IH // 2, 4):
                        n = min(4, IH // 2 - h0)
                        ps, pk = fm_group(cfg.OQI + 128 * h0, n)
                        rope_group(ps, pk, n, rt64, ck["rt64"], "c64", "s64",
                                   lambda m, h0=h0, t0=t0: QIT[h0 + m, :, t0:t0 + T])
                    ps, pk = tm_block(cfg.OWI, IH)
                    for k in range(NT):
                        P.op("act", lambda e, ps=ps, k=k: e.activation(out=wis[:, k * IH:(k + 1) * IH],
                                                                        in_=ps[:, k * 512:k * 512 + IH], func=AF.Copy,
                                                                        scale=float(IH ** -0.5 * 64 ** -0.5)),
                             reads=[pk], writes=[("wis", k)])
                        P.dma("sp", WI[t0 + 128 * k:t0 + 128 * (k + 1), :], wis[:, k * IH:(k + 1) * IH],
                              reads=[("wis", k)], writes=[("scr", "wi", k)])
                    for c0 in range(0, DS, 512):
                        n = min(512, DS - c0)
                        ps, pk = tm_block(cfg.OZ + c0, n)
                        for k in range(NT):
                            act_copy(zst[:, k * 512:k * 512 + n], ps[:, k * 512:k * 512 + n], [pk], [("zst", k)],
                                     func=AF.Silu)
                            P.dma("sp", ZS[t0 + 128 * k:t0 + 128 * (k + 1), c0:c0 + n], zst[:, k * 512:k * 512 + n],
                                  reads=[("zst", k)], writes=[("scr", "z", k)])
                    for (off, dst) in ((cfg.OGA, GA), (cfg.OGS, GS)):
                        for c0 in range(0, DC, 4):
                            n = min(4, DC - c0)
                            ps, pk = fm_group(off + 128 * c0, n)
                            act_copy(gst[:, 0:n * T], ps[:, 0:n * T], [pk], ["gst"], func=AF.Sigmoid)
                            P.dma("sp", dst[c0:c0 + n, :, t0:t0 + T].rearrange("c p t -> p c t"),
                                  gst[:, 0:n * T].rearrange("p (c t) -> p c t", c=n), reads=["gst"],
                                  writes=[("scr", "g")])
            P.barrier()
            P.stack = old

    if 3 in phases:
        with ExitStack() as ph:
            old = P.stack
            P.stack = ph
            NCH = S // 128
            xs_t = P.sbuf("xs_t", [128, DS], F32R)
            btok = P.sbuf("btok", [128, GN], F32R)
            btc = P.sbuf("btc", [128, SG * 128], F32R)
            ctc = P.sbuf("ctc", [128, SG * 128], F32R)
            dt_t = P.sbuf("dt_t", [128, SH])
            aneg = P.sbuf("aneg", [128, SH])
            sm = P.sbuf("sm", [128, 8 * SH])
            xdt = P.sbuf("xdt", [128, DS], F32R)
            xdtd = P.sbuf("xdtd", [128, DS], F32R)
            cbm = P.sbuf("cbm", [128, 128])
            dg4 = P.sbuf("dg4", [128, 512])
            seg4 = P.sbuf("seg4", [128, 512])
            M4 = P.sbuf("M4", [128, 512], F32R)
            H32 = P.sbuf("H32", [128, DS])
            Hr = P.sbuf("Hr", [128, DS], F32R)
            ybuf = [P.sbuf("y%d" % i, [128, DS]) for i in range(2)]
            ytmp = P.sbuf("ytmp", [128, DS])
            dsk = P.sbuf("dsk", [128, DS])
            yst = P.sbuf("yst", [128, 512], F32R)
            st3 = P.sbuf("st3", [128, 16])
            junk3 = P.sbuf("junk3", [128, 512], BF16)
            P.dma("sp", aneg[:, :], io["alog_b"][:, :], writes=["aneg"])
            act_copy(aneg[:, :], aneg[:, :], ["aneg"], ["aneg"], func=AF.Exp)
            dve_ts(aneg[:, :], aneg[:, :], -1.0, None, ALU.mult, None, ["aneg"], ["aneg"])
            P.dma("sp", dsk[:, :], io["dskip_b"][:, :], writes=["dsk"])
            P.op("dve", lambda e: e.memset(H32[:, :], 0.0), writes=[("H32", g) for g in range(SG)])
            P.op("dve", lambda e: e.memset(Hr[:, :].bitcast(F32), 0.0), writes=[("Hr", g) for g in range(SG)])
            adt, acs, eacs, fdec, cdec = [sm[:, i * SH:(i + 1) * SH] for i in range(5)]
            NGRP = DS // SG
            for c in range(NCH):
                r0 = c * 128
                P.dma("sp", xs_t[:, :], XS[r0:r0 + 128, :], writes=["xs_t"])
                P.dma("sp", btok[:, :], BTK[r0:r0 + 128, :], writes=["btok"])
                P.dma("sp", btc[:, :].rearrange("p (g l) -> p g l", g=SG),
                      BT[:, :, r0:r0 + 128].rearrange("g p l -> p g l"), writes=["btc"])
                P.dma("sp", ctc[:, :].rearrange("p (g l) -> p g l", g=SG),
                      CT[:, :, r0:r0 + 128].rearrange("g p l -> p g l"), writes=["ctc"])
                P.dma("sp", dt_t[:, :], DTs[r0:r0 + 128, :], writes=["dt_t"])
                dve_tt(adt, dt_t[:, :], aneg[:, :], ALU.mult, ["dt_t", "aneg"], ["adt"])
                ps, pk = next_tp()
                P.op("pe", lambda e, ps=ps: e.matmul(ps[:, 0:SH], lhsT=tri[:, :], rhs=adt, start=True, stop=True),
                     reads=["adt", ck["tri"]], writes=[pk])
                P.op("pe", lambda e, ps=ps: e.matmul(ps[:, SH:2 * SH], lhsT=ones[:, :], rhs=adt, start=False, stop=True,
                                                     skip_group_check=True),
                     reads=["adt", ck["ones"]], writes=[pk])
                act_copy(acs, ps[:, 0:SH], [pk], ["acs"])
                act_copy(cdec, ps[:, SH:2 * SH], [pk], ["cdec"], func=AF.Exp)
                dve_tt(fdec, ps[:, SH:2 * SH], acs, ALU.subtract, [pk, "acs"], ["fdec"])
                act_copy(fdec, fdec, ["fdec"], ["fdec"], func=AF.Exp)
                dve_tt(fdec, fdec, dt_t[:, :], ALU.mult, ["fdec", "dt_t"], ["fdec"])
                act_copy(eacs, acs, ["acs"], ["eacs"], func=AF.Exp)
                xs3 = xs_t[:, :].bitcast(F32).rearrange("p (h j) -> p h j", j=64)
                dve_tt(xdt[:, :].rearrange("p (h j) -> p h j", j=64), xs3,
                       dt_t[:, :].unsqueeze(2).to_broadcast([128, SH, 64]), ALU.mult, ["xs_t", "dt_t"], ["xdt"])
                dve_tt(xdtd[:, :].rearrange("p (h j) -> p h j", j=64), xs3,
                       fdec.unsqueeze(2).to_broadcast([128, SH, 64]), ALU.mult, ["xs_t", "fdec"], ["xdtd"])
                y = ybuf[c % 2]
                yk = ("y", c % 2)
                for g in range(SG):
                    cps, cpk = next_tp()
                    P.op("pe", lambda e, cps=cps, g=g: e.matmul(cps[:, 0:128], lhsT=btc[:, g * 128:(g + 1) * 128],
                                                                rhs=ctc[:, g * 128:(g + 1) * 128], start=True, stop=True),
                         reads=["btc", "ctc"], writes=[cpk])
                    dve_tt(cbm[:, :], cps[:, 0:128], tri[:, :], ALU.mult, [cpk, ck["tri"]], ["cbm"])
                    yps, ypk = next_mp()
                    firsty = True
                    for hb in range(0, R, 4):
                        nb = min(4, R - hb)
                        h0 = g * R + hb
                        P.op("dve", lambda e, h0=h0, nb=nb: e.tensor_tensor(
                            out=dg4[:, 0:nb * 128].rearrange("p (h l) -> p h l", h=nb),
                            in0=ident[:, :].unsqueeze(1).to_broadcast([128, nb, 128]),
                            in1=acs[:, h0:h0 + nb].unsqueeze(2).to_broadcast([128, nb, 128]), op=ALU.mult),
                             reads=["acs", ck["ident"]], writes=["dg4"])
                        rps, rpk = next_tp()
                        P.op("pe", lambda e, rps=rps, nb=nb: e.matmul(rps[:, 0:nb * 128], lhsT=ones[:, :],
                                                                      rhs=dg4[:, 0:nb * 128], start=True, stop=True),
                             reads=["dg4", ck["ones"]], writes=[rpk])
                        for hh in range(nb):
                            h = h0 + hh
                            dve_ts(seg4[:, hh * 128:(hh + 1) * 128], rps[:, hh * 128:(hh + 1) * 128], acs[:, h:h + 1], 0.0,
                                   ALU.subtract, ALU.min, [rpk, "acs"], ["seg4"])
                        act_copy(seg4[:, 0:nb * 128], seg4[:, 0:nb * 128], ["seg4"], ["seg4"], func=AF.Exp)
                        P.op("dve", lambda e, nb=nb: e.tensor_tensor(
                            out=M4[:, 0:nb * 128].rearrange("p (h l) -> p h l", h=nb),
                            in0=seg4[:, 0:nb * 128].rearrange("p (h l) -> p h l", h=nb),
                            in1=cbm[:, :].unsqueeze(1).to_broadcast([128, nb, 128]), op=ALU.mult),
                             reads=["seg4", "cbm"], writes=["M4"])
                        for hh in range(nb):
                            h = h0 + hh
                            hr = hb + hh
                            P.op("pe", lambda e, yps=yps, hh=hh, h=h, hr=hr, firsty=firsty: e.matmul(
                                yps[:, hr * 64:(hr + 1) * 64], lhsT=M4[:, hh * 128:(hh + 1) * 128],
                                rhs=xdt[:, h * 64:(h + 1) * 64], start=firsty, stop=True, skip_group_check=True),
                                 reads=["M4", "xdt"], writes=[ypk])
                            firsty = False
                    P.op("pe", lambda e, yps=yps, g=g: e.matmul(yps[:, 512:512 + RP], lhsT=ctc[:, g * 128:(g + 1) * 128],
                                                                rhs=Hr[:, g * RP:(g + 1) * RP], start=True, stop=True),
                         reads=["ctc", ("Hr", g)], writes=[ypk])
                    dve_tt(ytmp[:, g * RP:(g + 1) * RP].rearrange("p (h j) -> p h j", j=64),
                           yps[:, 512:512 + RP].rearrange("p (h j) -> p h j", j=64),
                           eacs[:, g * R:(g + 1) * R].unsqueeze(2).to_broadcast([128, R, 64]), ALU.mult,
                           [ypk, "eacs"], [("ytmp", g)])
                    dve_tt(y[:, g * RP:(g + 1) * RP], ytmp[:, g * RP:(g + 1) * RP], yps[:, 0:RP], ALU.add,
                           [ypk, ("ytmp", g)], [yk])
                    sps, spk = next_mp()
                    P.op("pe", lambda e, sps=sps, g=g: e.matmul(sps[:, 0:RP], lhsT=btok[:, g * 128:(g + 1) * 128],
                                                                rhs=xdtd[:, g * RP:(g + 1) * RP], start=True, stop=True),
                         reads=["btok", "xdtd"], writes=[spk])
                    Hg = H32[:, g * RP:(g + 1) * RP]
                    dve_tt(Hg.rearrange("p (h j) -> p h j", j=64), Hg.rearrange("p (h j) -> p h j", j=64),
                           cdec[:, g * R:(g + 1) * R].unsqueeze(2).to_broadcast([128, R, 64]), ALU.mult,
                           [("H32", g), "cdec"], [("H32", g)])
                    dve_tt(Hg, Hg, sps[:, 0:RP], ALU.add, [("H32", g), spk], [("H32", g)])
                    act_copy(Hr[:, g * RP:(g + 1) * RP], Hg, [("H32", g)], [("Hr", g)])
                dve_tt(ytmp[:, :], xs_t[:, :].bitcast(F32), dsk[:, :], ALU.mult, ["xs_t", "dsk"] +
                       [("ytmp", g) for g in range(SG)], [("ytmp", g) for g in range(SG)])
                dve_tt(y[:, :], y[:, :], ytmp[:, :], ALU.add, [yk] + [("ytmp", g) for g in range(SG)], [yk])
                if c % 2 == 1:
                    j = c // 2
                    y0, y1 = ybuf[0], ybuf[1]
                    dve_ts(ytmp[:, :], y0[:, :], sel[:, 0:1], None, ALU.mult, None,
                           [("y", 0), ck["sel"]] + [("ytmp", g) for g in range(SG)], [("ytmp", g) for g in range(SG)])
                    dve_stt(ytmp[:, :], y1[:, :], sel[:, 1:2], ytmp[:, :], ALU.mult, ALU.add,
                            [("y", 1), ck["sel"]] + [("ytmp", g) for g in range(SG)], [("ytmp", g) for g in range(SG)])
                    zs_t = ybuf[0]
                    sg_b = ybuf[1]
                    P.dma("sp", zs_t[:, :], ZS[j * 128:(j + 1) * 128, :], writes=[("y", 0)])
                    P.dma("sp", sg_b[:, :], io["ssdg_b"][:, :], writes=[("y", 1)])
                    yk_all = [("ytmp", g) for g in range(SG)]
                    dve_tt(ytmp[:, :], ytmp[:, :], zs_t[:, :], ALU.mult, yk_all + [("y", 0)], yk_all)
                    P.op("dve", lambda e: e.memset(st3[:, 0:SG], 0.0), writes=["st3"])
                    for gg in range(SG):
                        P.op("act", lambda e, gg=gg: e.activation(
                            out=junk3[:, 0:NGRP], in_=ytmp[:, gg * NGRP:(gg + 1) * NGRP], func=AF.Square,
                            accum_out=st3[:, gg:gg + 1]), reads=yk_all + ["st3"], writes=["junk3", "st3"])
                    rstd_from_ss(st3[:, 0:SG], "st3", 1.0 / NGRP)
                    dve_tt(ytmp[:, :].rearrange("p (g j) -> p g j", g=SG), ytmp[:, :].rearrange("p (g j) -> p g j", g=SG),
                           st3[:, 0:SG].unsqueeze(2).to_broadcast([128, SG, NGRP]), ALU.mult, yk_all + ["st3"], yk_all)
                    dve_tt(ytmp[:, :], ytmp[:, :], sg_b[:, :], ALU.mult, yk_all + [("y", 1)], yk_all)
                    for c0 in range(0, DS // 128, 4):
                        n = min(4, DS // 128 - c0)
                        rp, rk = next_tp()
                        for m in range(n):
                            P.op("pe", lambda e, rp=rp, m=m, c0=c0: e.transpose(
                                out=rp[:, m * 128:(m + 1) * 128], in_=ytmp[:, (c0 + m) * 128:(c0 + m + 1) * 128],
                                identity=ident[:, :]), reads=yk_all + [ck["ident"]], writes=[rk])
                        act_copy(yst[:, 0:n * 128], rp[:, 0:n * 128], [rk], ["yst"])
                        P.dma("sp", YT[c0:c0 + n, :, j * 128:(j + 1) * 128].rearrange("c p t -> p c t"),
                              yst[:, 0:n * 128].rearrange("p (c t) -> p c t", c=n), reads=["yst"], writes=[("scr", "yt")])
            P.barrier()
            P.stack = old

    if 4 in phases:
        with ExitStack() as ph:
            old = P.stack
            P.stack = ph
            NQB = cfg.NQB
            G = cfg.GRPQ
            scale = 128 ** -0.5
            iota = P.sbuf("iota", [128, S])
            qpos = P.sbuf("qpos", [128, NQB])
            qit = P.sbuf("qit", [128, (IH // 2) * 128], F32R)
            wi_t = P.sbuf("wi_t", [128, IH])
            score = P.sbuf("score", [128, S])
            selb = P.sbuf("selb", [128, S])
            rl = [P.sbuf("rl%d" % i, [128, 512]) for i in range(2)]
            p4 = P.sbuf("p4", [128, G * S])
            ktg = P.sbuf("ktg", [128, S], F32R)
            kit = ktg
            vg = P.sbuf("vg", [128, (S // 128) * 128], F32R)
            qt = P.sbuf("qt", [128, NH * 128], F32R)
            pts = [P.sbuf("pts%d" % i, [128, 512], F32R) for i in range(2)]
            ost = P.sbuf("ost", [128, 512], F32R)
            m8 = P.sbuf("m8", [128, 8])
            st4 = P.sbuf("st4", [128, 4 * G])
            P.dma("sp", iota[:, :], io["iota"][:, :], writes=["iota"])
            P.dma("sp", qpos[:, :], io["qpos"][:, :], writes=["qpos"])
            work = p4[:, 0:S]
            rli = 0
            pti = 0
            for j in range(NQB):
                SJ = 256 * (j + 1)
                NS = SJ // 128
                t0 = j * 128
                kblocks = [(b0, min(512, SJ - b0)) for b0 in range(0, SJ, 512)]
                P.dma("sp", qit[:, :].rearrange("p (c t) -> p c t", t=128),
                      QIT[:, :, t0:t0 + 128].rearrange("c p t -> p c t"), writes=["qit"])
                P.dma("sp", wi_t[:, :], WI[t0:t0 + 128, :], writes=["wi_t"])
                P.dma("sp", kit[:, 0:SJ], KIT[:, 0:SJ], writes=["ktg"])
                P.dma("sp", qt[:, :].rearrange("p (c t) -> p c t", t=128),
                      QT[:, :, t0:t0 + 128].rearrange("c p t -> p c t"), writes=["qt"])
                for (b0, bn) in kblocks:
                    for h in range(IH):
                        pb = (h % 2) * 64
                        ps, pk = next_tp()
                        P.op("pe", lambda e, ps=ps, h=h, pb=pb, b0=b0, bn=bn: e.matmul(
                            ps[:, 0:bn], lhsT=qit[pb:pb + 64, (h // 2) * 128:(h // 2 + 1) * 128],
                            rhs=kit[pb:pb + 64, b0:b0 + bn], start=True, stop=True), reads=["qit", "ktg"], writes=[pk])
                        r_ = rl[rli % 2]
                        rk_ = ("rl", rli % 2)
                        rli += 1
                        act_copy(r_[:, 0:bn], ps[:, 0:bn], [pk], [rk_], func=AF.Relu)
                        if h == 0:
                            dve_ts(score[:, b0:b0 + bn], r_[:, 0:bn], wi_t[:, 0:1], None, ALU.mult, None,
                                   [rk_, "wi_t"], [("score", b0)])
                        else:
                            dve_stt(score[:, b0:b0 + bn], r_[:, 0:bn], wi_t[:, h:h + 1], score[:, b0:b0 + bn], ALU.mult,
                                    ALU.add, [rk_, "wi_t", ("score", b0)], [("score", b0)])
                skeys = [("score", b0) for (b0, bn) in kblocks]
                dve_ts(selb[:, 0:SJ], iota[:, 0:SJ], qpos[:, j:j + 1], NEG, ALU.is_gt, ALU.mult, ["iota", "qpos"], ["selb"])
                dve_tt(score[:, 0:SJ], score[:, 0:SJ], selb[:, 0:SJ], ALU.add, skeys + ["selb"], skeys)
                P.op("dve", lambda e, SJ=SJ: e.tensor_copy(out=work[:, 0:SJ], in_=score[:, 0:SJ]), reads=skeys,
                     writes=[("p4", 0)])
                nround = cfg.TOPK // 8
                for r in range(nround):
                    P.op("dve", lambda e, SJ=SJ: e.max(out=m8[:, :], in_=work[:, 0:SJ]), reads=[("p4", 0)], writes=["m8"])
                    if r < nround - 1:
                        P.op("dve", lambda e, SJ=SJ: e.match_replace(out=work[:, 0:SJ], in_to_replace=m8[:, :],
                                                                    in_values=work[:, 0:SJ], imm_value=-3.0e38),
                             reads=[("p4", 0), "m8"], writes=[("p4", 0)])
                dve_ts(work[:, 0:SJ], score[:, 0:SJ], m8[:, 7:8], NEG, ALU.is_lt, ALU.mult, skeys + ["m8", ("p4", 0)], [("p4", 0)])
                dve_tt(selb[:, 0:SJ], selb[:, 0:SJ], work[:, 0:SJ], ALU.add, ["selb", ("p4", 0)], ["selb"])
                for g in range(NKV):
                    P.dma("sp", ktg[:, 0:SJ], KT[g, :, 0:SJ], writes=["ktg"])
                    P.dma("sp", vg[:, 0:NS * 128].rearrange("p (c d) -> p c d", d=128),
                          V[0:SJ, g * 128:(g + 1) * 128].rearrange("(c p) d -> p c d", p=128), writes=["vg"])
                    for hh in range(G):
                        h = g * G + hh
                        ph_ = p4[:, hh * S:hh * S + SJ]
                        phk = ("p4", hh)
                        for (b0, bn) in kblocks:
                            ps, pk = next_tp()
                            P.op("pe", lambda e, ps=ps, h=h, b0=b0, bn=bn: e.matmul(
                                ps[:, 0:bn], lhsT=qt[:, h * 128:(h + 1) * 128], rhs=ktg[:, b0:b0 + bn], start=True,
                                stop=True), reads=["qt", "ktg"], writes=[pk])
                            dve_stt(p4[:, hh * S + b0:hh * S + b0 + bn], ps[:, 0:bn], scale, selb[:, b0:b0 + bn], ALU.mult,
                                    ALU.add, [pk, "selb"], [phk])
                        P.op("dve", lambda e, ph_=ph_, hh=hh: e.reduce_max(out=st4[:, hh:hh + 1], in_=ph_, axis=AX.X),
                             reads=[phk], writes=[("mx", hh)])
                        dve_ts(st4[:, hh:hh + 1], st4[:, hh:hh + 1], -1.0, None, ALU.mult, None, [("mx", hh)], [("mx", hh)])
                        P.op("dve", lambda e, hh=hh: e.memset(st4[:, G + hh:G + hh + 1], 0.0), writes=[("sm", hh)])
                        P.op("act", lambda e, ph_=ph_, hh=hh: e.activation(
                            out=ph_, in_=ph_, func=AF.Exp, bias=st4[:, hh:hh + 1], scale=1.0,
                            accum_out=st4[:, G + hh:G + hh + 1]), reads=[phk, ("mx", hh), ("sm", hh)],
                             writes=[phk, ("sm", hh)])
                        P.op("dve", lambda e, hh=hh: e.reciprocal(out=st4[:, G + hh:G + hh + 1],
                                                                   in_=st4[:, G + hh:G + hh + 1]),
                             reads=[("sm", hh)], writes=[("sm", hh)])
                        dve_ts(ph_, ph_, st4[:, G + hh:G + hh + 1], None, ALU.mult, None, [phk, ("sm", hh)], [phk])
                    ops_, opk = next_mp()
                    for sc in range(NS):
                        rp, rk = next_tp()
                        for hh in range(G):
                            P.op("pe", lambda e, rp=rp, hh=hh, sc=sc: e.transpose(
                                out=rp[:, hh * 128:(hh + 1) * 128],
                                in_=p4[:, hh * S + sc * 128:hh * S + (sc + 1) * 128], identity=ident[:, :]),
                                 reads=[("p4", hh), ck["ident"]], writes=[rk])
                        pt_ = pts[pti % 2]
                        ptk = ("pts", pti % 2)
                        pti += 1
                        act_copy(pt_[:, 0:G * 128], rp[:, 0:G * 128], [rk], [ptk])
                        P.op("pe", lambda e, ops_=ops_, pt_=pt_, sc=sc, NS=NS: e.matmul(
                            ops_[:, 0:G * 128], lhsT=vg[:, sc * 128:(sc + 1) * 128], rhs=pt_[:, 0:G * 128],
                            start=(sc == 0), stop=(sc == NS - 1)), reads=["vg", ptk], writes=[opk])
                    act_copy(ost[:, 0:G * 128], ops_[:, 0:G * 128], [opk], ["ost"])
                    P.dma("sp", OT[g * G:(g + 1) * G, :, t0:t0 + 128].rearrange("c p t -> p c t"),
                          ost[:, 0:G * 128].rearrange("p (c t) -> p c t", c=G), reads=["ost"], writes=[("scr", "ot")])
            P.barrier()
            P.stack = old

    outs = []
    if 5 in phases:
        with ExitStack() as ph:
            old = P.stack
            P.stack = ph
            alloc_w()
            KA = cfg.DA // 128
            KS = DS // 128
            assert KS * T == NT * D and KA * T >= D // 2
            oT = P.sbuf("oT", [128, max(KA * T, D)], F32R)
            yT = P.sbuf("yT", [128, KS * T], F32R)
            Cm = P.sbuf("Cm", [128, DC * T], F32R)
            A5v = P.sbuf("A5", [128, NT * D])
            gpb = P.sbuf("gpb", [128, D])
            gat = P.sbuf("gat", [128, 4 * T])
            gst_ = P.sbuf("gst5", [128, 4 * T])
            mt = P.sbuf("mt", [128, 4 * T])
            st5v = P.sbuf("st5", [128, 8])
            P.dma("sp", gpb[:, :], io["gpost_b"][:, :], writes=["gpb"])
            y_out = io["y"]
            oTk = [("oT", c) for c in range(max(KA, D // T))]
            for tt in range(SO // T):
                t0 = tt * T
                P.dma("sp", oT[:, 0:KA * T].rearrange("p (c t) -> p c t", t=T),
                      OT[:, :, t0:t0 + T].rearrange("c p t -> p c t"), writes=oTk)
                P.dma("sp", yT[:, :].rearrange("p (c t) -> p c t", t=T), YT[:, :, t0:t0 + T].rearrange("c p t -> p c t"),
                      writes=[("yT", c) for c in range(KS)])
                for dg in range(0, DC, 4):
                    n = min(4, DC - dg)
                    res = []
                    for (wname, src, srck, KC) in (("w_attn_o", oT, "oT", KA), ("w_ssd_o", yT, "yT", KS)):
                        ps, pk = next_mp()
                        for kc in range(KC):
                            wb, wk = next_w()
                            P.dma("sp", wb[:, 0:128 * n], io[wname][kc * 128:(kc + 1) * 128, dg * 128:(dg + n) * 128],
                                  writes=[wk])
                            for m in range(n):
                                P.op("pe", lambda e, ps=ps, wb=wb, m=m, kc=kc, src=src, KC=KC: e.matmul(
                                    ps[:, m * T:(m + 1) * T], lhsT=wb[:, m * 128:(m + 1) * 128],
                                    rhs=src[:, kc * T:(kc + 1) * T], start=(kc == 0 and m % 2 == 0), stop=(kc == KC - 1),
                                    skip_group_check=True), reads=[wk, (srck, kc)], writes=[pk])
                        res.append((ps, pk))
                    P.dma("sp", gat[:, 0:n * T].rearrange("p (c t) -> p c t", c=n),
                          GA[dg:dg + n, :, t0:t0 + T].rearrange("c p t -> p c t"), writes=["gat"])
                    P.dma("sp", gst_[:, 0:n * T].rearrange("p (c t) -> p c t", c=n),
                          GS[dg:dg + n, :, t0:t0 + T].rearrange("c p t -> p c t"), writes=["gst5"])
                    dve_tt(mt[:, 0:n * T], res[0][0][:, 0:n * T], gat[:, 0:n * T], ALU.mult, [res[0][1], "gat"], ["mt"])
                    dve_tt(gst_[:, 0:n * T], res[1][0][:, 0:n * T], gst_[:, 0:n * T], ALU.mult, [res[1][1], "gst5"],
                           ["gst5"])
                    dve_tt(Cm[:, dg * T:(dg + n) * T], mt[:, 0:n * T], gst_[:, 0:n * T], ALU.add, ["mt", "gst5"],
                           [("Cm", dg + m) for m in range(n)])
                Bo = yT
                for dg in range(0, DC, 4):
                    n = min(4, DC - dg)
                    ps, pk = next_mp()
                    for kc in range(DC):
                        wb, wk = next_w()
                        P.dma("sp", wb[:, 0:128 * n], io["w_out"][kc * 128:(kc + 1) * 128, dg * 128:(dg + n) * 128],
                              writes=[wk])
                        for m in range(n):
                            P.op("pe", lambda e, ps=ps, wb=wb, m=m, kc=kc: e.matmul(
                                ps[:, m * T:(m + 1) * T], lhsT=wb[:, m * 128:(m + 1) * 128],
                                rhs=Cm[:, kc * T:(kc + 1) * T], start=(kc == 0 and m % 2 == 0), stop=(kc == DC - 1),
                                skip_group_check=True), reads=[wk, ("Cm", kc)], writes=[pk])
                    act_copy(Bo[:, dg * T:(dg + n) * T], ps[:, 0:n * T], [pk], [("yT", dg + m) for m in range(n)])
                for k in range(NT):
                    Ak = A5v[:, k * D:(k + 1) * D]
                    xkeys = [("Cm", i) for i in range(k * D // T, (k + 1) * D // T)]
                    P.dma("sp", Cm[:, k * D:(k + 1) * D], io["xor"][t0 + 128 * k:t0 + 128 * (k + 1), :], writes=xkeys)
                    Xk = Cm[:, k * D:(k + 1) * D].bitcast(F32)
                    for cg in range(0, DC, 4):
                        n = min(4, DC - cg)
                        rp, rk = next_tp()
                        for cc in range(n):
                            c = cg + cc
                            P.op("pe", lambda e, rp=rp, cc=cc, c=c, k=k: e.transpose(
                                out=rp[:, cc * 128:(cc + 1) * 128],
                                in_=Bo[:, c * T + k * 128:c * T + (k + 1) * 128].bitcast(F32), identity=ident[:, :]),
                                 reads=[("yT", c), ck["ident"]], writes=[rk])
                        act_copy(A5v[:, k * D + cg * 128:k * D + (cg + n) * 128], rp[:, 0:n * 128], [rk], [("A5", k)])
                    P.op("dve", lambda e, k=k: e.memset(st5v[:, k:k + 1], 0.0), writes=[("st5", k)])
                    P.op("act", lambda e, Ak=Ak, k=k: e.activation(out=oT[:, 0:D], in_=Ak, func=AF.Square,
                                                                  accum_out=st5v[:, k:k + 1]),
                         reads=[("A5", k), ("st5", k)], writes=oTk + [("st5", k)])
                    rstd_from_ss(st5v[:, k:k + 1], ("st5", k), 1.0 / D)
                    dve_stt(Ak, Ak, st5v[:, k:k + 1], gpb[:, :], ALU.mult, ALU.mult, [("A5", k), ("st5", k), "gpb"],
                            [("A5", k)])
                    dve_tt(Ak, Ak, Xk, ALU.add, [("A5", k)] + xkeys, [("A5", k)])
                    P.dma("sp", y_out[t0 + 128 * k:t0 + 128 * (k + 1), :], Ak, reads=[("A5", k)], writes=[("yout", tt, k)])
                    outs.append(("yout", tt, k))
            P.stack = old
    return outs, dict(KT=KT, V=V, KIT=KIT, DTs=DTs, XS=XS, BTK=BTK, BT=BT, CT=CT, QT=QT, QIT=QIT, WI=WI, ZS=ZS, GA=GA,
                      GS=GS, YT=YT, OT=OT)


def emit_ffn(P, nc, x, xr, w13, w2, g1c, g2b, ident_d, y, Tc, D, FF, dbg=None):
    T = 256
    NT = T // 128
    DC = D // 128
    FC = FF // 128
    NW = 8
    EPS = 1e-6
    A = P.sbuf("A", [128, NT * D])
    B = P.sbuf("B", [128, DC * T], F32R)
    C = P.sbuf("C", [128, FC * T], F32R)
    junk = P.sbuf("junk", [128, D], mybir.dt.bfloat16)
    g1s = P.sbuf("g1s", [128, DC])
    g2s = P.sbuf("g2s", [128, D])
    ident = P.sbuf("ident", [128, 128])
    W = [P.sbuf("W%d" % i, [128, 512], F32R) for i in range(NW)]
    sa = P.sbuf("sa", [128, 4 * T])
    st = P.sbuf("st", [128, 8])
    tp = [P.psum("tp%d" % i, [128, 512]) for i in range(2)]
    mp = [P.psum("mp%d" % i, [128, 1024]) for i in range(3)]

    P.dma("sp", g1s[:, :], g1c[:, :], writes=["g1s"])
    P.dma("sp", g2s[:, :], g2b[:, :], writes=["g2s"])
    P.dma("sp", ident[:, :], ident_d[:, :], writes=["ident"])

    wi = 0
    tpi = 0
    mpi = 0
    for tt in range(Tc // T):
        t0 = tt * T
        for k in range(NT):
            Ak = A[:, k * D:(k + 1) * D]
            P.dma("sp", Ak, x[t0 + 128 * k:t0 + 128 * (k + 1), :], writes=[("A", k)])
            P.op("dve", lambda e, k=k: e.memset(st[:, k:k + 1], 0.0), writes=[("st", k)])
            P.op("act", lambda e, Ak=Ak, k=k: e.activation(out=junk[:, :], in_=Ak, func=AF.Square,
                                                          accum_out=st[:, k:k + 1]),
                 reads=[("A", k)], writes=["junk", ("st", k)])
            P.op("dve", lambda e, k=k: e.tensor_scalar(out=st[:, k:k + 1], in0=st[:, k:k + 1],
                                                       scalar1=1.0 / D, scalar2=EPS, op0=ALU.mult, op1=ALU.add),
                 reads=[("st", k)], writes=[("st", k)])
            P.op("act", lambda e, k=k: e.activation(out=st[:, k:k + 1], in_=st[:, k:k + 1], func=AF.Sqrt),
                 reads=[("st", k)], writes=[("st", k)])
            P.op("dve", lambda e, k=k: e.reciprocal(out=st[:, k:k + 1], in_=st[:, k:k + 1]),
                 reads=[("st", k)], writes=[("st", k)])
            P.op("dve", lambda e, Ak=Ak, k=k: e.tensor_scalar(out=Ak, in0=Ak, scalar1=st[:, k:k + 1],
                                                            scalar2=None, op0=ALU.mult),
                 reads=[("A", k), ("st", k)], writes=[("A", k)])
        for c2 in range(DC // 2):
            ps = tp[tpi % 2]
            pk = ("tp", tpi % 2)
            tpi += 1
            for cc in range(2):
                c = 2 * c2 + cc
                for k in range(NT):
                    P.op("pe", lambda e, ps=ps, cc=cc, k=k, c=c: e.transpose(
                        out=ps[:, (cc * NT + k) * 128:(cc * NT + k + 1) * 128],
                        in_=A[:, k * D + c * 128:k * D + (c + 1) * 128], identity=ident[:, :]),
                         reads=[("A", k), "ident"], writes=[pk])
            for cc in range(2):
                c = 2 * c2 + cc
                P.op("dve", lambda e, ps=ps, cc=cc, c=c: e.tensor_scalar(
                    out=B[:, c * T:(c + 1) * T], in0=ps[:, cc * T:(cc + 1) * T],
                    scalar1=g1s[:, c:c + 1], scalar2=None, op0=ALU.mult),
                     reads=[pk, "g1s"], writes=[("B", c)])
        if dbg is not None:
            P.dma("sp", dbg[0][:, :], B[:, :].bitcast(F32), reads=[("B", c) for c in range(DC)], writes=["dbg0"])
        for j in range(FF // 512):
            for half in range(2):
                col0 = half * FF + 512 * j
                ps = mp[mpi % 3]
                pk = ("mp", mpi % 3)
                mpi += 1
                for dk in range(DC):
                    wb = W[wi % NW]
                    wk = ("W", wi % NW)
                    wi += 1
                    P.dma("sp", wb[:, :], w13[dk * 128:(dk + 1) * 128, col0:col0 + 512], writes=[wk])
                    for m in range(4):
                        P.op("pe", lambda e, ps=ps, wb=wb, m=m, dk=dk: e.matmul(
                            ps[:, m * T:(m + 1) * T], lhsT=wb[:, m * 128:(m + 1) * 128],
                            rhs=B[:, dk * T:(dk + 1) * T], start=(dk == 0 and m % 2 == 0), stop=(dk == DC - 1), skip_group_check=True),
                             reads=[wk, ("B", dk)], writes=[pk])
                if half == 0:
                    P.op("act", lambda e, ps=ps: e.activation(out=sa[:, :], in_=ps[:, :], func=AF.Silu),
                         reads=[pk], writes=["sa"])
                else:
                    P.op("dve", lambda e, ps=ps, j=j: e.tensor_tensor(
                        out=C[:, 4 * j * T:(4 * j + 4) * T], in0=sa[:, :], in1=ps[:, :], op=ALU.mult),
                         reads=[pk, "sa"], writes=[("C", 4 * j + m) for m in range(4)])
        if dbg is not None:
            P.dma("sp", dbg[1][:, :], C[:, :].bitcast(F32), reads=[("C", c) for c in range(FC)], writes=["dbg1"])
        for dg in range(D // 512):
            ps = mp[mpi % 3]
            pk = ("mp", mpi % 3)
            mpi += 1
            for fk in range(FC):
                wb = W[wi % NW]
                wk = ("W", wi % NW)
                wi += 1
                P.dma("sp", wb[:, :], w2[fk * 128:(fk + 1) * 128, dg * 512:(dg + 1) * 512], writes=[wk])
                for m in range(4):
                    P.op("pe", lambda e, ps=ps, wb=wb, m=m, fk=fk: e.matmul(
                        ps[:, m * T:(m + 1) * T], lhsT=wb[:, m * 128:(m + 1) * 128],
                        rhs=C[:, fk * T:(fk + 1) * T], start=(fk == 0 and m % 2 == 0), stop=(fk == FC - 1), skip_group_check=True),
                         reads=[wk, ("C", fk)], writes=[pk])
            P.op("act", lambda e, ps=ps, dg=dg: e.activation(out=B[:, 4 * dg * T:(4 * dg + 4) * T], in_=ps[:, :],
                                                            func=AF.Copy),
                 reads=[pk], writes=[("B", 4 * dg + m) for m in range(4)])
        if dbg is not None:
            P.dma("sp", dbg[2][:, :], B[:, :].bitcast(F32), reads=[("B", c) for c in range(DC)], writes=["dbg2"])
        cx_keys = lambda k: [("C", i) for i in range(k * D // T, (k + 1) * D // T)]
        for k in range(NT):
            Ak = A[:, k * D:(k + 1) * D]
            Cx = C[:, k * D:(k + 1) * D].bitcast(F32)
            P.dma("sp", C[:, k * D:(k + 1) * D], xr[t0 + 128 * k:t0 + 128 * (k + 1), :], writes=cx_keys(k))
            for cg in range(DC // 4):
                ps = tp[tpi % 2]
                pk = ("tp", tpi % 2)
                tpi += 1
                for cc in range(4):
                    c = 4 * cg + cc
                    P.op("pe", lambda e, ps=ps, cc=cc, c=c, k=k: e.transpose(
                        out=ps[:, cc * 128:(cc + 1) * 128],
                        in_=B[:, c * T + k * 128:c * T + (k + 1) * 128].bitcast(F32), identity=ident[:, :]),
                         reads=[("B", c), "ident"], writes=[pk])
                P.op("act", lambda e, ps=ps, cg=cg, k=k: e.activation(
                    out=A[:, k * D + cg * 512:k * D + (cg + 1) * 512], in_=ps[:, :], func=AF.Copy),
                     reads=[pk], writes=[("A", k)])
            P.op("dve", lambda e, k=k: e.memset(st[:, 4 + k:5 + k], 0.0), writes=[("st2", k)])
            P.op("act", lambda e, Ak=Ak, k=k: e.activation(out=junk[:, :], in_=Ak, func=AF.Square,
                                                          accum_out=st[:, 4 + k:5 + k]),
                 reads=[("A", k)], writes=["junk", ("st2", k)])
            P.op("dve", lambda e, k=k: e.tensor_scalar(out=st[:, 4 + k:5 + k], in0=st[:, 4 + k:5 + k],
                                                       scalar1=1.0 / D, scalar2=EPS, op0=ALU.mult, op1=ALU.add),
                 reads=[("st2", k)], writes=[("st2", k)])
            P.op("act", lambda e, k=k: e.activation(out=st[:, 4 + k:5 + k], in_=st[:, 4 + k:5 + k], func=AF.Sqrt, scale=4.0),
                 reads=[("st2", k)], writes=[("st2", k)])
            P.op("dve", lambda e, k=k: e.reciprocal(out=st[:, 4 + k:5 + k], in_=st[:, 4 + k:5 + k]),
                 reads=[("st2", k)], writes=[("st2", k)])
            P.op("dve", lambda e, Ak=Ak, k=k: e.scalar_tensor_tensor(
                out=Ak, in0=Ak, scalar=st[:, 4 + k:5 + k], in1=g2s[:, :], op0=ALU.mult, op1=ALU.mult),
                 reads=[("A", k), ("st2", k), "g2s"], writes=[("A", k)])
            P.op("dve", lambda e, Ak=Ak, Cx=Cx: e.tensor_tensor(out=Ak, in0=Ak, in1=Cx, op=ALU.add),
                 reads=[("A", k)] + cx_keys(k), writes=[("A", k)])
            P.dma("sp", y[t0 + 128 * k:t0 + 128 * (k + 1), :], Ak, reads=[("A", k)], writes=[("y", tt, k)])
    return [("y", tt, k) for tt in range(Tc // T) for k in range(NT)]


from concourse.bass_utils import run_bass_kernel_spmd

NCORES = 8
_CACHE = {}


def build_ffn_nc(Tc, D, FF):
    nc = bass.Bass("TRN2", target_bir_lowering=False)
    nc.dge_precook = False
    x = nc.dram_tensor("x", [Tc, D], F32, kind="ExternalInput").ap()
    xr = nc.dram_tensor("xr", [Tc, D], F32R, kind="ExternalInput").ap()
    w13 = nc.dram_tensor("w13", [D, 2 * FF], F32R, kind="ExternalInput").ap()
    w2 = nc.dram_tensor("w2", [FF, D], F32R, kind="ExternalInput").ap()
    g1c = nc.dram_tensor("g1c", [128, D // 128], F32, kind="ExternalInput").ap()
    g2b = nc.dram_tensor("g2b", [128, D], F32, kind="ExternalInput").ap()
    ident = nc.dram_tensor("ident", [128, 128], F32, kind="ExternalInput").ap()
    y = nc.dram_tensor("y", [Tc, D], F32, kind="ExternalOutput").ap()
    with ExitStack() as stack:
        P = Prog(nc, stack)
        outs = emit_ffn(P, nc, x, xr, w13, w2, g1c, g2b, ident, y, Tc, D, FF)
        P.wait_all("sp", outs)
        P.emit()
    return nc


MIX_R32 = {"w_in", "w_attn_o", "w_ssd_o", "w_out", "rt128", "rt64", "xor"}


def build_mixer_nc(cfg, example):
    nc = bass.Bass("TRN2", target_bir_lowering=False)
    nc.dge_precook = False
    io = {}
    for name, arr in example.items():
        dt_ = I32 if arr.dtype == np.int32 else (F32R if name in MIX_R32 else F32)
        io[name] = nc.dram_tensor(name, list(arr.shape), dt_, kind="ExternalInput").ap()
    io["y"] = nc.dram_tensor("y", [cfg.SO, cfg.D], F32, kind="ExternalOutput").ap()
    with ExitStack() as stack:
        P = Prog(nc, stack)
        outs, _ = emit_mixer(P, nc, cfg, io)
        P.wait_all("sp", outs)
        P.emit()
    return nc


def _own_rows(cfg, h):
    return np.concatenate([np.arange(128) + 128 * (2 * j + h) for j in range(cfg.NQB)])


def _split_own(cfg, xfull):
    out = []
    for c in range(NCORES):
        b, h = c // 2, c % 2
        out.append(np.ascontiguousarray(xfull[b][_own_rows(cfg, h)]))
    return out


def _merge_own(cfg, parts, B):
    xfull = np.empty((B, cfg.S, cfg.D), np.float32)
    for c in range(NCORES):
        b, h = c // 2, c % 2
        xfull[b][_own_rows(cfg, h)] = parts[c]
    return xfull


def run_ffn(cfg, xfull, w13, w2, g1, g2, FF):
    key = ("ffn", cfg.SO, cfg.D, FF)
    if key not in _CACHE:
        _CACHE[key] = build_ffn_nc(cfg.SO, cfg.D, FF)
    nc = _CACHE[key]
    xs = _split_own(cfg, xfull)
    g1c = cols(g1)
    g2b = bc(g2)
    ident = np.eye(128, dtype=np.float32)
    in_maps = [{"x": xs[c], "xr": xs[c], "w13": w13, "w2": w2, "g1c": g1c, "g2b": g2b, "ident": ident}
               for c in range(NCORES)]
    res = run_bass_kernel_spmd(nc, in_maps, core_ids=list(range(NCORES)))
    return _merge_own(cfg, [res.results[c]["y"] for c in range(NCORES)], xfull.shape[0])


def run_mixer(cfg, xfull, positions, l, p):
    in_maps = []
    for c in range(NCORES):
        b, h = c // 2, c % 2
        in_maps.append(host_inputs(cfg, h, xfull[b], positions[b], p["mix_pre_g"][l], p["w_in"][l], p["idx_ln_g"][l],
                                   p["idx_ln_b"][l], p["conv_w"][l], p["conv_b"][l], p["dt_bias"][l], p["a_log"][l],
                                   p["d_skip"][l], p["ssd_norm_g"][l], p["w_attn_o"][l], p["w_ssd_o"][l], p["w_out"][l],
                                   p["mix_post_g"][l]))
    key = ("mix",)
    if key not in _CACHE:
        _CACHE[key] = build_mixer_nc(cfg, in_maps[0])
    nc = _CACHE[key]
    res = run_bass_kernel_spmd(nc, in_maps, core_ids=list(range(NCORES)))
    return _merge_own(cfg, [res.results[c]["y"] for c in range(NCORES)], xfull.shape[0])


def kernel(**inputs):
    p = {k: np.asarray(v) for k, v in inputs.items()}
    cfg = Cfg()
    x = np.asarray(p["x"], np.float32)
    positions = np.asarray(p["positions"], np.int32)
    depth = p["w_in"].shape[0]
    FF = p["ffn1_w2"].shape[1]
    for l in range(depth):
        x = run_ffn(cfg, x, p["ffn1_w13"][l], p["ffn1_w2"][l], p["ffn1_pre_g"][l], p["ffn1_post_g"][l], FF)
        x = run_mixer(cfg, x, positions, l, p)
        x = run_ffn(cfg, x, p["ffn2_w13"][l], p["ffn2_w2"][l], p["ffn2_pre_g"][l], p["ffn2_post_g"][l], FF)
    return x
```
